# Optimizing a Trainium2 kernel written in Bass

```python
import jax, jax.numpy as jnp
from jax import lax
import numpy as np

D_MODEL = 4096
BATCH = 4
SEQ = 4096
DEPTH = 2

BLOCK = 128
RMS_EPS = 1e-6
MLA_HEADS = 16
Q_LORA = 1536
KV_LORA = 512
NOPE_DIM = 128
ROPE_DIM = 64
MLA_V_DIM = 128
ROPE_THETA = 10000.0
FOX_HEADS = 16
FOX_HEAD_DIM = 128
SWA_Q_HEADS = 32
SWA_KV_HEADS = 4
SWA_HEAD_DIM = 64
WINDOW = 128
N_BRANCHES = 3
MLA_WIDTH = MLA_HEADS * MLA_V_DIM
FOX_WIDTH = FOX_HEADS * FOX_HEAD_DIM
SWA_WIDTH = SWA_Q_HEADS * SWA_HEAD_DIM
SWA_KV_WIDTH = SWA_KV_HEADS * SWA_HEAD_DIM
MIX_WIDTH = MLA_WIDTH + FOX_WIDTH + SWA_WIDTH
IN_SIZES = (Q_LORA, KV_LORA, ROPE_DIM,
            FOX_WIDTH, FOX_WIDTH, FOX_WIDTH, FOX_HEADS,
            SWA_WIDTH, SWA_KV_WIDTH, SWA_KV_WIDTH,
            N_BRANCHES * D_MODEL)
IN_WIDTH = sum(IN_SIZES)
D_FF_DENSE = 14336
N_EXPERTS = 8
TOP_K = 2
D_FF_EXPERT = 4096
N_DENSE_LAYERS = (DEPTH + 1) // 2
N_MOE_LAYERS = DEPTH // 2

kernel_name = 'hybrid_mla_fox_swa_gated_moe'


def _split_points(sizes):
    pts, acc = [], 0
    for s in sizes[:-1]:
        acc += s
        pts.append(acc)
    return pts


def rms_norm(x, g):
    x32 = x.astype(jnp.float32)
    y = x32 * lax.rsqrt(jnp.mean(x32 * x32, axis=-1, keepdims=True) + RMS_EPS)
    return (y * g.astype(jnp.float32)).astype(x.dtype)


def rope(x, pos):
    half = x.shape[-1] // 2
    inv = ROPE_THETA ** (-jnp.arange(half, dtype=jnp.float32) / half)
    ang = pos.astype(jnp.float32)[:, None] * inv[None, :]
    cos = jnp.cos(ang)[:, None, :]
    sin = jnp.sin(ang)[:, None, :]
    x32 = x.astype(jnp.float32)
    x1, x2 = x32[..., :half], x32[..., half:]
    return jnp.concatenate([x1 * cos - x2 * sin, x2 * cos + x1 * sin], axis=-1).astype(x.dtype)


def alibi_slopes(n):
    return jnp.exp2(-8.0 * jnp.arange(1, n + 1, dtype=jnp.float32) / n)


def causal_block_attention(q, k, v, scale, cum=None):
    B, S, H, dk = q.shape
    dv = v.shape[-1]
    nb = S // BLOCK
    qb = q.reshape(B, nb, BLOCK, H, dk).transpose(1, 0, 2, 3, 4)
    key_pos = jnp.arange(S)
    cum_k = None if cum is None else cum.transpose(0, 2, 1)

    def attend(i, q_blk, cum_blk):
        s = jnp.einsum('bqhd,bkhd->bhqk', q_blk, k, preferred_element_type=jnp.float32) * scale
        if cum_blk is not None:
            s = s + cum_blk[..., None] - cum_k[:, :, None, :]
        q_pos = i * BLOCK + jnp.arange(BLOCK)
        mask = q_pos[:, None] >= key_pos[None, :]
        p = jax.nn.softmax(jnp.where(mask, s, -jnp.inf), axis=-1)
        return jnp.einsum('bhqk,bkhd->bqhd', p.astype(v.dtype), v)

    idx = jnp.arange(nb)
    if cum is None:
        out = lax.map(lambda a: attend(a[0], a[1], None), (idx, qb))
    else:
        cb = cum.reshape(B, nb, BLOCK, H).transpose(1, 0, 3, 2)
        out = lax.map(lambda a: attend(a[0], a[1], a[2]), (idx, qb, cb))
    return out.transpose(1, 0, 2, 3, 4).reshape(B, S, H * dv)


def sliding_window_sink_attention(q, k, v, sinks, slopes):
    B, S, Hkv, G, d = q.shape
    nb = S // BLOCK
    qb = q.reshape(B, nb, BLOCK, Hkv, G, d)

    def band(t):
        prev = jnp.pad(t, ((0, 0), (BLOCK, 0), (0, 0), (0, 0)))[:, :S]
        return jnp.concatenate([prev.reshape(B, nb, BLOCK, Hkv, d),
                                t.reshape(B, nb, BLOCK, Hkv, d)], axis=2)

    kb, vb = band(k), band(v)
    s = jnp.einsum('bnqhgd,bnkhd->bnhgqk', qb, kb, preferred_element_type=jnp.float32) * (d ** -0.5)
    dist = jnp.arange(BLOCK)[:, None] + BLOCK - jnp.arange(2 * BLOCK)[None, :]
    key_pos = (jnp.arange(nb) * BLOCK - BLOCK)[:, None] + jnp.arange(2 * BLOCK)[None, :]
    valid = (dist >= 0)[None] & (dist < WINDOW)[None] & (key_pos >= 0)[:, None, :]
    alibi = -slopes.astype(jnp.float32).reshape(Hkv, G)[:, :, None, None] * dist.astype(jnp.float32)
    s = jnp.where(valid[None, :, None, None], s + alibi, -jnp.inf)
    sink = jnp.broadcast_to(sinks.astype(jnp.float32).reshape(1, 1, Hkv, G, 1, 1), s.shape[:-1] + (1,))
    p = jax.nn.softmax(jnp.concatenate([s, sink], axis=-1), axis=-1)[..., :-1]
    o = jnp.einsum('bnhgqk,bnkhd->bnqhgd', p.astype(v.dtype), vb)
    return o.reshape(B, S, Hkv * G * d)


def hybrid_mixer(xn, w_in, b_forget, b_gate, g_q, g_kv, w_uq, w_ukv, sinks, w_branch, w_out):
    B, S, _ = xn.shape
    pos = jnp.arange(S)
    h = xn @ w_in
    (c_q, c_kv, k_r, fq, fk, fv, f_logit, sq, sk, sv, gate_logit) = jnp.split(
        h, _split_points(IN_SIZES), axis=-1)

    q = (rms_norm(c_q, g_q) @ w_uq).reshape(B, S, MLA_HEADS, NOPE_DIM + ROPE_DIM)
    q = jnp.concatenate([q[..., :NOPE_DIM], rope(q[..., NOPE_DIM:], pos)], axis=-1)
    kv = (rms_norm(c_kv, g_kv) @ w_ukv).reshape(B, S, MLA_HEADS, NOPE_DIM + MLA_V_DIM)
    k_rope = jnp.broadcast_to(rope(k_r[:, :, None, :], pos), (B, S, MLA_HEADS, ROPE_DIM))
    k = jnp.concatenate([kv[..., :NOPE_DIM], k_rope], axis=-1)
    o_mla = causal_block_attention(q, k, kv[..., NOPE_DIM:], (NOPE_DIM + ROPE_DIM) ** -0.5)

    log_f = jax.nn.log_sigmoid((f_logit + b_forget).astype(jnp.float32))
    cum = jnp.cumsum(log_f, axis=1)
    fox_shape = (B, S, FOX_HEADS, FOX_HEAD_DIM)
    o_fox = causal_block_attention(fq.reshape(fox_shape), fk.reshape(fox_shape), fv.reshape(fox_shape),
                                   FOX_HEAD_DIM ** -0.5, cum)

    g = SWA_Q_HEADS // SWA_KV_HEADS
    o_swa = sliding_window_sink_attention(
        sq.reshape(B, S, SWA_KV_HEADS, g, SWA_HEAD_DIM),
        sk.reshape(B, S, SWA_KV_HEADS, SWA_HEAD_DIM),
        sv.reshape(B, S, SWA_KV_HEADS, SWA_HEAD_DIM),
        sinks, alibi_slopes(SWA_Q_HEADS))

    y_mla = o_mla @ w_branch[:MLA_WIDTH]
    y_fox = o_fox @ w_branch[MLA_WIDTH:MLA_WIDTH + FOX_WIDTH]
    y_swa = o_swa @ w_branch[MLA_WIDTH + FOX_WIDTH:]
    gt_mla, gt_fox, gt_swa = jnp.split(jax.nn.sigmoid(gate_logit + b_gate), N_BRANCHES, axis=-1)
    return (gt_mla * y_mla + gt_fox * y_fox + gt_swa * y_swa) @ w_out


def swiglu(x, w_gate, w_up, w_down):
    return (jax.nn.silu(x @ w_gate) * (x @ w_up)) @ w_down


def moe_swiglu(xn, w_router, w_gate, w_up, w_down):
    logits = jnp.einsum('bsd,de->bse', xn, w_router, preferred_element_type=jnp.float32)
    top_v, top_i = lax.top_k(logits, TOP_K)
    weights = jax.nn.softmax(top_v, axis=-1)
    combine = jnp.einsum('bsk,bske->bse', weights,
                         jax.nn.one_hot(top_i, N_EXPERTS, dtype=jnp.float32))
    out = jnp.zeros_like(xn)
    for e in range(N_EXPERTS):
        out = out + combine[..., e:e + 1].astype(xn.dtype) * swiglu(xn, w_gate[e], w_up[e], w_down[e])
    return out


def _normal(key, shape, scale):
    return jax.random.normal(key, shape, jnp.float32) * scale


def setup_inputs(seed: int = 0) -> dict:
    key = jax.random.key(seed)
    ks = jax.random.split(key, 24)
    L, D = DEPTH, D_MODEL
    w_branch = jnp.concatenate([
        _normal(ks[9], (L, MLA_WIDTH, D), MLA_WIDTH ** -0.5),
        _normal(ks[10], (L, FOX_WIDTH, D), FOX_WIDTH ** -0.5),
        _normal(ks[11], (L, SWA_WIDTH, D), SWA_WIDTH ** -0.5)], axis=1)
    return {
        'x': jax.random.normal(ks[0], (BATCH, SEQ, D), jnp.float32),
        'g_mix_norm': 1.0 + _normal(ks[1], (L, D), 0.1),
        'w_in': _normal(ks[2], (L, D, IN_WIDTH), D ** -0.5),
        'b_forget': jax.random.uniform(ks[3], (L, FOX_HEADS), jnp.float32, 1.0, 4.0),
        'b_gate': _normal(ks[4], (L, N_BRANCHES * D), 0.1),
        'g_q_norm': 1.0 + _normal(ks[5], (L, Q_LORA), 0.1),
        'g_kv_norm': 1.0 + _normal(ks[6], (L, KV_LORA), 0.1),
        'w_uq': _normal(ks[7], (L, Q_LORA, MLA_HEADS * (NOPE_DIM + ROPE_DIM)), Q_LORA ** -0.5),
        'w_ukv': _normal(ks[8], (L, KV_LORA, MLA_HEADS * (NOPE_DIM + MLA_V_DIM)), KV_LORA ** -0.5),
        'sinks': _normal(ks[12], (L, SWA_Q_HEADS), 1.0),
        'w_branch': w_branch,
        'w_out': _normal(ks[13], (L, D, D), D ** -0.5),
        'g_ffn_norm': 1.0 + _normal(ks[14], (L, D), 0.1),
        'w_dense_gate': _normal(ks[15], (N_DENSE_LAYERS, D, D_FF_DENSE), D ** -0.5),
        'w_dense_up': _normal(ks[16], (N_DENSE_LAYERS, D, D_FF_DENSE), D ** -0.5),
        'w_dense_down': _normal(ks[17], (N_DENSE_LAYERS, D_FF_DENSE, D), D_FF_DENSE ** -0.5),
        'w_router': _normal(ks[18], (N_MOE_LAYERS, D, N_EXPERTS), D ** -0.5),
        'w_exp_gate': _normal(ks[19], (N_MOE_LAYERS, N_EXPERTS, D, D_FF_EXPERT), D ** -0.5),
        'w_exp_up': _normal(ks[20], (N_MOE_LAYERS, N_EXPERTS, D, D_FF_EXPERT), D ** -0.5),
        'w_exp_down': _normal(ks[21], (N_MOE_LAYERS, N_EXPERTS, D_FF_EXPERT, D), D_FF_EXPERT ** -0.5),
        'g_final': 1.0 + _normal(ks[22], (D,), 0.1),
    }


def reference(x, g_mix_norm, w_in, b_forget, b_gate, g_q_norm, g_kv_norm, w_uq, w_ukv, sinks,
              w_branch, w_out, g_ffn_norm, w_dense_gate, w_dense_up, w_dense_down,
              w_router, w_exp_gate, w_exp_up, w_exp_down, g_final):
    for l in range(DEPTH):
        xn = rms_norm(x, g_mix_norm[l])
        x = x + hybrid_mixer(xn, w_in[l], b_forget[l], b_gate[l], g_q_norm[l], g_kv_norm[l],
                             w_uq[l], w_ukv[l], sinks[l], w_branch[l], w_out[l])
        xn = rms_norm(x, g_ffn_norm[l])
        j = l // 2
        if l % 2 == 0:
            x = x + swiglu(xn, w_dense_gate[j], w_dense_up[j], w_dense_down[j])
        else:
            x = x + moe_swiglu(xn, w_router[j], w_exp_gate[j], w_exp_up[j], w_exp_down[j])
    return rms_norm(x, g_final)
```

```python
import math
from contextlib import ExitStack

import numpy as np
import concourse.bass as bass
import concourse.mybir as mybir
from concourse.bass_utils import run_bass_kernel_spmd

F32 = mybir.dt.float32
BF16 = mybir.dt.bfloat16
AF = mybir.ActivationFunctionType
ALU = mybir.AluOpType

D = 4096
NQ, NKV, NR = 1536, 512, 64
IN_W = 23120
OFF_CQ, OFF_CKV, OFF_KR, OFF_FQ, OFF_FK, OFF_FV, OFF_FL, OFF_SQ, OFF_SK, OFF_SV, OFF_G = (
    0, 1536, 2048, 2112, 4160, 6208, 8256, 8272, 10320, 10576, 10832)
EPS = 1e-6
TB = 512
NTB = TB // 128
N_CORES_USED = 4


class Buf:
    __slots__ = ("w", "r")

    def __init__(self):
        self.w = []
        self.r = {}


class Eng:
    def __init__(self, name, h, sem):
        self.name, self.h, self.sem, self.cnt, self.waited = name, h, sem, 0, {}


class Em:
    def __init__(self, nc, es, nslots=48):
        self.nc = nc
        self.E = {}
        for name, h in (("pe", nc.tensor), ("act", nc.scalar), ("dve", nc.vector),
                        ("pool", nc.gpsimd), ("sp", nc.sync)):
            self.E[name] = Eng(name, h, es.enter_context(nc.semaphore("s_" + name)))
        self.slots = [[es.enter_context(nc.semaphore("d%d" % i)), 0] for i in range(nslots)]
        self.nsw = 16
        self.si = {"sw": 0, "hw": 0}

    def _wait(self, e, reads, writes, join=False):
        deps = []
        for b in reads:
            deps.extend((t, True) for t in b.w)
        for b in writes:
            if not join:
                deps.extend((t, False) for t in b.w)
            deps.extend((t, False) for t in b.r.values())
        for ((sem, key, val, en), raw) in deps:
            if e.name == "pe" and en == "pe":
                continue
            if en == e.name and en in ("act", "dve") and not raw:
                continue
            if e.waited.get(key, 0) >= val:
                continue
            e.h.wait_ge(sem, val)
            e.waited[key] = val

    def _mark(self, tok, key, reads, writes, join=False):
        for b in reads:
            b.r[key] = tok
        for b in writes:
            if join:
                b.w.append(tok)
            else:
                b.w = [tok]
                b.r = {}

    def op(self, en, fns, reads=(), writes=()):
        e = self.E[en]
        self._wait(e, reads, writes)
        if callable(fns):
            fns = [fns]
        ins = None
        for f in fns:
            ins = f(e.h)
        e.cnt += 1
        ins.then_inc(e.sem, 1)
        self._mark((e.sem, en, e.cnt, en), en, reads, writes)

    def dma(self, qn, out, in_, reads=(), writes=(), join=False):
        e = self.E[qn]
        self._wait(e, reads, writes, join)
        if qn == "pool":
            idx = self.si["sw"]
            self.si["sw"] = (idx + 1) % self.nsw
        else:
            idx = self.nsw + self.si["hw"]
            self.si["hw"] = (self.si["hw"] + 1) % (len(self.slots) - self.nsw)
        sl = self.slots[idx]
        key = "d%d" % idx
        if sl[1] > 0 and e.waited.get(key, 0) < sl[1]:
            e.h.wait_ge(sl[0], sl[1])
            e.waited[key] = sl[1]
        sl[1] += 16
        e.h.dma_start(out=out, in_=in_).then_inc(sl[0], 16)
        self._mark((sl[0], key, sl[1], "dma"), key, reads, writes, join)

    def barrier(self):
        toks = [(e.sem, n, e.cnt, n) for n, e in self.E.items() if e.cnt > 0]
        toks += [(sl[0], "d%d" % i, sl[1], "dma") for i, sl in enumerate(self.slots) if sl[1] > 0]
        for e in self.E.values():
            for (sem, key, val, en) in toks:
                if en == e.name or e.waited.get(key, 0) >= val:
                    continue
                e.h.wait_ge(sem, val)
                e.waited[key] = val


class Ring:
    def __init__(self, items):
        self.items, self.i = items, 0

    def next(self):
        it = self.items[self.i]
        self.i = (self.i + 1) % len(self.items)
        return it


def build(S, n_layers=2, do_ffn=(True, True), do_final=True, stop=0):
    NT = S // 128
    NB = S // TB
    nc = bass.Bass("TRN2", target_bir_lowering=False)

    def din(name, shape, dt=F32):
        return nc.dram_tensor(name, list(shape), dt, kind="ExternalInput").ap()

    def dscr(name, shape, dt=BF16):
        return nc.dram_tensor(name, list(shape), dt).ap()

    x_in = din("x", [S, D])
    W = {}
    for l in range(n_layers):
        W[l] = dict(
            g_mix=din(f"g_mix{l}", [D]), w_in=din(f"w_in{l}", [D, IN_W]), b_forget=din(f"b_forget{l}", [16]),
            b_gate=din(f"b_gate{l}", [3 * D]), g_q=din(f"g_q{l}", [NQ]), g_kv=din(f"g_kv{l}", [NKV]),
            w_uq=din(f"w_uq{l}", [NQ, 3072]), w_ukv=din(f"w_ukv{l}", [NKV, 4096]), sinks=din(f"sinks{l}", [32]),
            w_branch=din(f"w_branch{l}", [6144, D]), w_out=din(f"w_out{l}", [D, D]), g_ffn=din(f"g_ffn{l}", [D]))
    if n_layers > 0 and do_ffn[0]:
        wdg, wdu, wdd = din("w_dg", [D, 14336]), din("w_du", [D, 14336]), din("w_dd", [14336, D])
    if n_layers > 1 and do_ffn[1]:
        wr = din("w_router", [D, 8])
        weg, weu, wed = din("w_eg", [8, D, 4096]), din("w_eu", [8, D, 4096]), din("w_ed", [8, 4096, D])
    g_final = din("g_final", [D])
    c_ident, c_tri, c_d0, c_d1, c_sel = (din(n, [128, 128]) for n in ("c_ident", "c_tri", "c_d0", "c_d1", "c_sel"))
    c_rope = din("c_rope", [S, 64])
    c_negm = din("c_negm", [128, 7 * 128])
    y_out = nc.dram_tensor("y", [S, D], F32, kind="ExternalOutput").ap()

    xa, xb, xc = dscr("xa", [S, D], F32), dscr("xb", [S, D], F32), dscr("xc", [S, D], F32)
    qnT, qrT, knT = dscr("qnT", [16, 128, S]), dscr("qrT", [16, 64, S]), dscr("knT", [16, 128, S])
    krT = dscr("krT", [64, S])
    vm, fv, sv = dscr("vm", [S, 2048]), dscr("fv", [S, 2048]), dscr("sv", [S, 256])
    fqT, fkT = dscr("fqT", [16, 128, S]), dscr("fkT", [16, 128, S])
    sqT, skT = dscr("sqT", [16, 128, S]), dscr("skT", [2, 128, S])
    gt = dscr("gt", [S, 3 * D])
    oT = dscr("oT", [48, 128, S])

    es = ExitStack()
    with es:
        K = Em(nc, es)

        uid = [0]

        def sb(stack, name, shape, dt):
            uid[0] += 1
            return stack.enter_context(nc.sbuf_tensor("%s_%d" % (name, uid[0]), list(shape), dt))

        psf = [(es.enter_context(nc.psum_tensor(f"psf{i}", [128, 512], F32)), Buf()) for i in range(6)]
        psb = [(es.enter_context(nc.psum_tensor(f"psb{i}", [128, 1024], BF16)), Buf()) for i in range(2)]
        psf_r, psb_r = Ring(psf), Ring(psb)
        ident = sb(es, "ident", [128, 128], BF16)
        ident32 = sb(es, "ident32", [128, 128], F32)
        tri = sb(es, "tri", [128, 128], BF16)
        tri32 = sb(es, "tri32", [128, 128], F32)
        ones32 = sb(es, "ones32", [128, 128], F32)
        ones_bf = sb(es, "ones_bf", [128, 128], BF16)
        sel32 = sb(es, "sel32", [128, 128], F32)
        d0 = sb(es, "d0", [128, 128], F32)
        d1 = sb(es, "d1", [128, 128], F32)
        rope = sb(es, "rope", [128, NT, 64], F32)
        Lall = sb(es, "Lall", [128, NT, 16], F32)
        ncum = sb(es, "ncum", [128, NT, 16], F32)
        nmid = sb(es, "nmid", [128, NT * 16], F32)
        cst = Buf()
        Lb, ncb, nmb = Buf(), Buf(), Buf()
        K.dma("pool", ident[:], c_ident[:, :], writes=[cst])
        K.dma("sp", ident32[:], c_ident[:, :], writes=[cst], join=True)
        K.dma("pool", tri[:], c_tri[:, :], writes=[cst], join=True)
        K.dma("sp", tri32[:], c_tri[:, :], writes=[cst], join=True)
        K.dma("sp", sel32[:], c_sel[:, :], writes=[cst], join=True)
        K.dma("sp", d0[:], c_d0[:, :], writes=[cst], join=True)
        K.dma("sp", d1[:], c_d1[:, :], writes=[cst], join=True)
        K.dma("sp", rope[:], c_rope.rearrange("(n p) c -> p n c", p=128), writes=[cst], join=True)
        ob = Buf()
        K.op("dve", lambda h: h.memset(ones32[:], 1.0), writes=[ob])
        K.op("dve", lambda h: h.memset(ones_bf[:], 1.0), writes=[ob])
        K.barrier()

        def transposes(src_tile, src_buf, pieces, dst_fn):
            for g0 in range(0, len(pieces), 8):
                grp = pieces[g0:g0 + 8]
                pt, pbuf = psb_r.next()
                fns = []
                for j, (c0, ncol) in enumerate(grp):
                    fns.append(lambda h, j=j, c0=c0, ncol=ncol: h.transpose(
                        out=pt[0:ncol, j * 128:(j + 1) * 128], in_=src_tile[:, c0:c0 + ncol], identity=ident[:]))
                K.op("pe", fns, reads=[src_buf, cst], writes=[pbuf])
                dst_fn(g0, len(grp), pt, pbuf)

        def rope_apply(ps, pbuf, c0, out_ap, obuf, tig, tmp, tbuf):
            cos, sin = rope[:, tig, 0:32], rope[:, tig, 32:64]
            x1, x2 = ps[:, c0:c0 + 32], ps[:, c0 + 32:c0 + 64]
            K.op("dve", lambda h: h.tensor_tensor(out=tmp[:, 0:32], in0=x1, in1=cos, op=ALU.mult), reads=[pbuf, cst], writes=[tbuf])
            K.op("dve", lambda h: h.tensor_tensor(out=tmp[:, 32:64], in0=x2, in1=sin, op=ALU.mult), reads=[pbuf, cst], writes=[tbuf])
            K.op("dve", lambda h: h.tensor_tensor(out=tmp[:, 64:96], in0=x2, in1=cos, op=ALU.mult), reads=[pbuf, cst], writes=[tbuf])
            K.op("dve", lambda h: h.tensor_tensor(out=tmp[:, 96:128], in0=x1, in1=sin, op=ALU.mult), reads=[pbuf, cst], writes=[tbuf])
            K.op("dve", lambda h: h.tensor_tensor(out=out_ap[:, 0:32], in0=tmp[:, 0:32], in1=tmp[:, 32:64], op=ALU.subtract), reads=[tbuf], writes=[obuf])
            K.op("dve", lambda h: h.tensor_tensor(out=out_ap[:, 32:64], in0=tmp[:, 64:96], in1=tmp[:, 96:128], op=ALU.add), reads=[tbuf], writes=[obuf])

        wcache = {}

        def get_cache(tag, n, KC):
            if tag not in wcache:
                wcache[tag] = (dscr("cw_" + tag, [n, 128, KC, 512]), [Buf() for _ in range(n)])
            return wcache[tag]

        def gemm(XT, XTbuf, KC, nt, chunks, epi, wring, cache=None, fp=True, ci0=0):
            for ci, ch in enumerate(chunks):
                wt, wb = wring.next()
                if cache is not None and not fp:
                    K.dma("sp", wt[:, 0:KC, :], cache[0][ci0 + ci], reads=[cache[1][ci0 + ci]], writes=[wb])
                else:
                    first = True
                    for (ap, off, c) in ch["parts"]:
                        src = ap.rearrange("(kc p) c -> p kc c", p=128)
                        K.dma("pool", wt[:, 0:KC, off:off + c], src[:, 0:KC, :], writes=[wb], join=not first)
                        first = False
                    if cache is not None:
                        K.dma("sp", cache[0][ci0 + ci], wt[:, 0:KC, :], reads=[wb], writes=[cache[1][ci0 + ci]])
                n = ch["ncols"]
                for ti in range(nt):
                    ps, pb = psf_r.next()
                    fns = [(lambda h, kc=kc: h.matmul(ps[:, 0:n], lhsT=XT[:, kc, ti * 128:(ti + 1) * 128],
                                                      rhs=wt[:, kc, 0:n], start=(kc == 0), stop=(kc == KC - 1)))
                           for kc in range(KC)]
                    K.op("pe", fns, reads=[XTbuf, wb], writes=[pb])
                    epi(ch, ti, ps, pb)

        def norm_rows(stack, x_src, g_ap, tb, XT, XTb, tag, want32=None):
            gbc = sb(stack, "gbc" + tag, [128, D], F32)
            xt = sb(stack, "xt" + tag, [128, D], F32)
            xn = sb(stack, "xn" + tag, [128, D], BF16)
            ss = sb(stack, "ss" + tag, [128, 2], F32)
            gb, xtb, xnb, ssb = Buf(), Buf(), Buf(), Buf()
            K.dma("sp", gbc[:], g_ap.partition_broadcast(128), writes=[gb])
            for ti in range(NTB):
                r0 = tb * TB + ti * 128
                K.dma("sp", xt[:], x_src[r0:r0 + 128, :], writes=[xtb])
                K.op("dve", lambda h: h.memset(ss[:], 0.0), writes=[ssb])
                K.op("act", lambda h: h.activation(out=xn[:], in_=xt[:], func=AF.Square, accum_out=ss[:, 0:1]),
                     reads=[xtb], writes=[xnb, ssb])
                K.op("act", lambda h: h.activation(out=ss[:, 1:2], in_=ss[:, 0:1], func=AF.Ln, scale=1.0 / D, bias=EPS),
                     reads=[ssb], writes=[ssb])
                K.op("act", lambda h: h.activation(out=ss[:, 1:2], in_=ss[:, 1:2], func=AF.Exp, scale=-0.5),
                     reads=[ssb], writes=[ssb])
                if want32 is not None:
                    want32(ti, xt, xtb, ss, ssb, gbc, gb)
                K.op("dve", lambda h: h.scalar_tensor_tensor(out=xn[:], in0=xt[:], scalar=ss[:, 1:2], in1=gbc[:],
                                                             op0=ALU.mult, op1=ALU.mult),
                     reads=[xtb, ssb, gb], writes=[xnb])

                def dst(j0, n, pt, pbuf, ti=ti):
                    K.op("act", lambda h: h.activation(
                        out=XT[:, j0:j0 + n, ti * 128:(ti + 1) * 128],
                        in_=pt[:, 0:n * 128].rearrange("p (j t) -> p j t", t=128), func=AF.Copy),
                        reads=[pbuf], writes=[XTb])
                transposes(xn, xnb, [(kc * 128, 128) for kc in range(32)], dst)

        def mixer(l, x_src, x_dst):
            w = W[l]
            with ExitStack() as ps_:
                XT = sb(ps_, "XT", [128, 32, TB], BF16)
                XTb = Buf()
                wts = Ring([(sb(ps_, f"wt{i}", [128, 32, 512], BF16), Buf()) for i in range(2)])
                cqT = sb(ps_, "cqT", [128, 16, TB], BF16)
                cqTb = Buf()
                gq = sb(ps_, "gq", [128, 2048], F32)
                gqb = Buf()
                bfb = sb(ps_, "bfb", [128, 16], F32)
                bfbb = Buf()
                ss2 = sb(ps_, "ss2", [128, 4], F32)
                ss2b = Buf()
                K.dma("sp", gq[:, 0:NQ], w["g_q"].partition_broadcast(128), writes=[gqb])
                K.dma("sp", gq[:, NQ:2048], w["g_kv"].partition_broadcast(128), writes=[gqb], join=True)
                K.dma("sp", bfb[:], w["b_forget"].partition_broadcast(128), writes=[bfbb])

                def seg_chunks(kind, col0, width, wap, **kw):
                    out = []
                    for c0 in range(0, width, 512):
                        c = min(512, width - c0)
                        out.append(dict(kind=kind, parts=[(wap[:, col0 + c0:col0 + c0 + c], 0, c)], ncols=c, c0=c0, **kw))
                    return out

                chunks = (seg_chunks("cq", OFF_CQ, 2048, w["w_in"]) + seg_chunks("kr", OFF_KR, 64, w["w_in"])
                          + seg_chunks("feat", OFF_FQ, 2048, w["w_in"], dst=fqT)
                          + seg_chunks("feat", OFF_FK, 2048, w["w_in"], dst=fkT)
                          + seg_chunks("tok", OFF_FV, 2048, w["w_in"], dst=fv)
                          + seg_chunks("fl", OFF_FL, 16, w["w_in"])
                          + seg_chunks("feat", OFF_SQ, 2048, w["w_in"], dst=sqT)
                          + seg_chunks("feat", OFF_SK, 256, w["w_in"], dst=skT)
                          + seg_chunks("tok", OFF_SV, 256, w["w_in"], dst=sv)
                          + seg_chunks("gate", OFF_G, 3 * D, w["w_in"]))

                for tb in range(NB):
                    t0 = tb * TB
                    with ExitStack() as ns:
                        norm_rows(ns, x_src, w["g_mix"], tb, XT, XTb, "m")
                        K.barrier()
                    if stop == 12:
                        return
                    es2 = ExitStack()
                    cq = sb(es2, "cq", [128, NTB, 2048], F32)
                    cqb = Buf()
                    cqn = sb(es2, "cqn", [128, 2048], BF16)
                    cqnb = Buf()
                    bg = Ring([(sb(es2, f"bg{i}", [128, 512], F32), Buf()) for i in range(2)])
                    st32 = Ring([(sb(es2, f"st32_{i}", [128, 512], F32), Buf()) for i in range(2)])
                    stbf = Ring([(sb(es2, f"stbf_{i}", [128, 512], BF16), Buf()) for i in range(3)])
                    stT = Ring([(sb(es2, f"stT_{i}", [128, 4, TB], BF16), Buf()) for i in range(2)])
                    cur = {}

                    def feat_store(ch, ti, src_tile, src_buf, dst, hidx0):
                        n = ch["ncols"] // 128
                        if ti == 0:
                            cur["stT"] = stT.next()
                        sT, sTb = cur["stT"]

                        def dstf(j0, nn, pt, pbuf):
                            K.op("dve", lambda h: h.tensor_copy(
                                out=sT[:, j0:j0 + nn, ti * 128:(ti + 1) * 128],
                                in_=pt[:, 0:nn * 128].rearrange("p (j t) -> p j t", t=128)),
                                reads=[pbuf], writes=[sTb])
                        transposes(src_tile, src_buf, [(j * 128, 128) for j in range(n)], dstf)
                        if ti == NTB - 1:
                            K.dma("sp", dst[hidx0:hidx0 + n, :, t0:t0 + TB].rearrange("h p t -> p h t"),
                                  sT[:, 0:n, :], reads=[sTb])

                    def epi(ch, ti, ps, pb):
                        kind, n = ch["kind"], ch["ncols"]
                        tig = tb * NTB + ti
                        r0 = t0 + ti * 128
                        if kind == "cq":
                            c0 = ch["c0"]
                            K.op("act", lambda h: h.activation(out=cq[:, ti, c0:c0 + n], in_=ps[:, 0:n], func=AF.Copy),
                                 reads=[pb], writes=[cqb])
                            if c0 + n == 2048:
                                K.op("dve", lambda h: h.memset(ss2[:], 0.0), writes=[ss2b])
                                K.op("act", lambda h: h.activation(out=cqn[:, 0:NQ], in_=cq[:, ti, 0:NQ], func=AF.Square,
                                                                   accum_out=ss2[:, 0:1]), reads=[cqb], writes=[cqnb, ss2b])
                                K.op("act", lambda h: h.activation(out=cqn[:, NQ:2048], in_=cq[:, ti, NQ:2048], func=AF.Square,
                                                                   accum_out=ss2[:, 1:2]), reads=[cqb], writes=[cqnb, ss2b])
                                for j, dim in ((0, NQ), (1, NKV)):
                                    K.op("act", lambda h, j=j, dim=dim: h.activation(
                                        out=ss2[:, 2 + j:3 + j], in_=ss2[:, j:j + 1], func=AF.Ln, scale=1.0 / dim, bias=EPS),
                                        reads=[ss2b], writes=[ss2b])
                                    K.op("act", lambda h, j=j: h.activation(
                                        out=ss2[:, 2 + j:3 + j], in_=ss2[:, 2 + j:3 + j], func=AF.Exp, scale=-0.5),
                                        reads=[ss2b], writes=[ss2b])
                                K.op("dve", lambda h: h.scalar_tensor_tensor(
                                    out=cqn[:, 0:NQ], in0=cq[:, ti, 0:NQ], scalar=ss2[:, 2:3], in1=gq[:, 0:NQ],
                                    op0=ALU.mult, op1=ALU.mult), reads=[cqb, ss2b, gqb], writes=[cqnb])
                                K.op("dve", lambda h: h.scalar_tensor_tensor(
                                    out=cqn[:, NQ:2048], in0=cq[:, ti, NQ:2048], scalar=ss2[:, 3:4], in1=gq[:, NQ:2048],
                                    op0=ALU.mult, op1=ALU.mult), reads=[cqb, ss2b, gqb], writes=[cqnb])

                                def dstq(j0, nn, pt, pbuf):
                                    K.op("act", lambda h: h.activation(
                                        out=cqT[:, j0:j0 + nn, ti * 128:(ti + 1) * 128],
                                        in_=pt[:, 0:nn * 128].rearrange("p (j t) -> p j t", t=128), func=AF.Copy),
                                        reads=[pbuf], writes=[cqTb])
                                transposes(cqn, cqnb, [(j * 128, 128) for j in range(16)], dstq)
                        elif kind == "kr":
                            o, obf = stbf.next()
                            t_, tb_ = st32.next()
                            rope_apply(ps, pb, 0, o[:, 0:64], obf, tig, t_, tb_)
                            if ti == 0:
                                cur["stT"] = stT.next()
                            sT, sTb = cur["stT"]

                            def dstk(j0, nn, pt, pbuf):
                                K.op("dve", lambda h: h.tensor_copy(out=sT[0:64, 0, ti * 128:(ti + 1) * 128], in_=pt[0:64, 0:128]),
                                     reads=[pbuf], writes=[sTb])
                            transposes(o, obf, [(0, 64)], dstk)
                            if ti == NTB - 1:
                                K.dma("sp", krT[:, t0:t0 + TB], sT[0:64, 0, :], reads=[sTb])
                        elif kind == "feat":
                            o, obf = stbf.next()
                            K.op("act", lambda h: h.activation(out=o[:, 0:n], in_=ps[:, 0:n], func=AF.Copy), reads=[pb], writes=[obf])
                            feat_store(ch, ti, o, obf, ch["dst"], ch["c0"] // 128)
                        elif kind == "tok":
                            o, obf = stbf.next()
                            K.op("act", lambda h: h.activation(out=o[:, 0:n], in_=ps[:, 0:n], func=AF.Copy), reads=[pb], writes=[obf])
                            c0 = ch["c0"]
                            K.dma("sp", ch["dst"][r0:r0 + 128, c0:c0 + n], o[:, 0:n], reads=[obf])
                        elif kind == "fl":
                            t_, tb_ = st32.next()
                            K.op("dve", lambda h: h.tensor_tensor(out=t_[:, 0:16], in0=ps[:, 0:16], in1=bfb[:], op=ALU.add),
                                 reads=[pb, bfbb], writes=[tb_])
                            K.op("act", lambda h: h.activation(out=t_[:, 16:32], in_=t_[:, 0:16], func=AF.Exp, scale=-1.0),
                                 reads=[tb_], writes=[tb_])
                            K.op("act", lambda h: h.activation(out=Lall[:, tig, :], in_=t_[:, 16:32], func=AF.Ln, bias=1.0),
                                 reads=[tb_], writes=[Lb])
                        elif kind == "gate":
                            c0 = ch["c0"]
                            if ti == 0:
                                cur["bg"] = bg.next()
                                K.dma("sp", cur["bg"][0][:], w["b_gate"][c0:c0 + 512].partition_broadcast(128), writes=[cur["bg"][1]])
                            bgt, bgb = cur["bg"]
                            t_, tb_ = st32.next()
                            K.op("dve", lambda h: h.tensor_tensor(out=t_[:], in0=ps[:], in1=bgt[:], op=ALU.add),
                                 reads=[pb, bgb], writes=[tb_])
                            o, obf = stbf.next()
                            K.op("act", lambda h: h.activation(out=o[:], in_=t_[:], func=AF.Sigmoid), reads=[tb_], writes=[obf])
                            K.dma("sp", gt[r0:r0 + 128, c0:c0 + 512], o[:], reads=[obf])

                    gemm(XT, XTb, 32, NTB, chunks[0:stop - 100] if stop > 100 else chunks, epi, wts,
                         cache=get_cache(f"win{l}", len(chunks), 32), fp=(tb == 0))
                    if stop == 13 or stop > 100:
                        K.barrier()
                        es2.close()
                        return

                    qch = [dict(parts=[(w["w_uq"][:, c * 384:(c + 1) * 384], 0, 384)], ncols=384, c=c) for c in range(8)]
                    curq = {}

                    def epi_q(ch, ti, ps, pb):
                        tig = tb * NTB + ti
                        c = ch["c"]
                        o, obf = stbf.next()
                        t_, tb_ = st32.next()
                        if stop == 15:
                            K.op("act", lambda h: h.activation(out=o[:, 0:384], in_=ps[:, 0:384], func=AF.Copy), reads=[pb], writes=[obf])
                            return
                        if ti == 0 and c < 3:
                            K.op("dve", lambda h: h.memset(o[:], 0.0), writes=[obf])
                        for hh in range(2):
                            b0 = hh * 192
                            K.op("dve", lambda h, b0=b0, hh=hh: h.tensor_copy(out=o[:, hh * 256:hh * 256 + 128], in_=ps[:, b0:b0 + 128]),
                                 reads=[pb], writes=[obf])
                            if stop != 16:
                                rope_apply(ps, pb, b0 + 128, o[:, hh * 256 + 128:hh * 256 + 192], obf, tig, t_[:, hh * 128:(hh + 1) * 128], tb_)
                        if stop in (16, 17):
                            return
                        if ti == 0:
                            curq["n"], curq["r"] = stT.next(), stT.next()
                        (sN, sNb), (sR, sRb) = curq["n"], curq["r"]

                        def dstq2(j0, nn, pt, pbuf):
                            for hh in range(2):
                                K.op("dve", lambda h, hh=hh: h.tensor_copy(out=sN[:, hh, ti * 128:(ti + 1) * 128],
                                                                           in_=pt[:, (2 * hh) * 128:(2 * hh + 1) * 128]),
                                     reads=[pbuf], writes=[sNb])
                                K.op("dve", lambda h, hh=hh: h.tensor_copy(out=sR[0:64, hh, ti * 128:(ti + 1) * 128],
                                                                           in_=pt[0:64, (2 * hh + 1) * 128:(2 * hh + 2) * 128]),
                                     reads=[pbuf], writes=[sRb])
                        transposes(o, obf, [(0, 128), (128, 128), (256, 128), (384, 128)], dstq2)
                        if stop == 18:
                            return
                        if ti == NTB - 1:
                            K.dma("sp", qnT[2 * c:2 * c + 2, :, t0:t0 + TB].rearrange("h p t -> p h t"), sN[:, 0:2, :], reads=[sNb])
                            K.dma("sp", qrT[2 * c:2 * c + 2, :, t0:t0 + TB].rearrange("h p t -> p h t"), sR[0:64, 0:2, :], reads=[sRb])

                    gemm(cqT, cqTb, 12, NTB, qch, epi_q, wts, cache=get_cache(f"uq{l}", 8, 12), fp=(tb == 0))
                    if stop in (14, 15, 16, 17, 18):
                        K.barrier()
                        es2.close()
                        return

                    kch = [dict(parts=[(w["w_ukv"][:, c * 512:(c + 1) * 512], 0, 512)], ncols=512, c=c) for c in range(8)]
                    curk = {}
                    cqT_kv = cqT[:, 12:16, :]

                    def epi_kv(ch, ti, ps, pb):
                        c = ch["c"]
                        r0 = t0 + ti * 128
                        o, obf = stbf.next()
                        K.op("act", lambda h: h.activation(out=o[:], in_=ps[:], func=AF.Copy), reads=[pb], writes=[obf])
                        K.dma("sp", vm[r0:r0 + 128, (2 * c) * 128:(2 * c + 2) * 128].rearrange("p (h d) -> p h d", d=128),
                              o[:].rearrange("p (h x d) -> p h x d", x=2, d=128)[:, :, 1, :], reads=[obf])
                        if ti == 0:
                            curk["s"] = stT.next()
                        sT, sTb = curk["s"]

                        def dstk2(j0, nn, pt, pbuf):
                            K.op("dve", lambda h: h.tensor_copy(out=sT[:, 0:2, ti * 128:(ti + 1) * 128],
                                                                in_=pt[:, 0:256].rearrange("p (j t) -> p j t", t=128)),
                                 reads=[pbuf], writes=[sTb])
                        transposes(o, obf, [(0, 128), (256, 128)], dstk2)
                        if ti == NTB - 1:
                            K.dma("sp", knT[2 * c:2 * c + 2, :, t0:t0 + TB].rearrange("h p t -> p h t"), sT[:, 0:2, :], reads=[sTb])

                    gemm(cqT_kv, cqTb, 4, NTB, kch, epi_kv, wts, cache=get_cache(f"ukv{l}", 8, 4), fp=(tb == 0))
                    K.barrier()
                    es2.close()
                K.barrier()

            if stop == 1:
                return
            for i in range(NT):
                ps, pb = psf_r.next()
                fns = [(lambda h, j=j: h.matmul(ps[:, 0:16], lhsT=ones32[:], rhs=Lall[:, j, :], start=(j == 0), stop=False))
                       for j in range(i)]
                fns.append(lambda h: h.matmul(ps[:, 0:16], lhsT=tri32[:], rhs=Lall[:, i, :], start=(i == 0), stop=True))
                K.op("pe", fns, reads=[Lb, cst, ob], writes=[pb])
                K.op("dve", lambda h: h.tensor_copy(out=ncum[:, i, :], in_=ps[:, 0:16]), reads=[pb], writes=[ncb])
            ps, pb = psf_r.next()
            K.op("pe", lambda h: h.matmul(ps[:, 0:NT * 16], lhsT=sel32[:], rhs=ncum[:].rearrange("p n h -> p (n h)"),
                                          start=True, stop=True), reads=[ncb, cst], writes=[pb])
            K.op("dve", lambda h: h.tensor_copy(out=nmid[:], in_=ps[:, 0:NT * 16]), reads=[pb], writes=[nmb])
            K.barrier()

            if stop == 2:
                return
            with ExitStack() as ps_:
                qn_r = Ring([(sb(ps_, f"aq{i}", [128, S], BF16), Buf()) for i in range(2)])
                kn_r = Ring([(sb(ps_, f"ak{i}", [128, S], BF16), Buf()) for i in range(2)])
                qr_r = Ring([(sb(ps_, f"aqr{i}", [64, S], BF16), Buf()) for i in range(2)])
                v_r = Ring([(sb(ps_, f"av{i}", [128, NT, 128], BF16), Buf()) for i in range(2)])
                o_r = Ring([(sb(ps_, f"ao{i}", [128, S], BF16), Buf()) for i in range(2)])
                kr = sb(ps_, "akr", [64, S], BF16)
                krb = Buf()
                pt_r = Ring([(sb(ps_, f"apt{i}", [128, 512], BF16), Buf()) for i in range(4)])
                rd_r = Ring([(sb(ps_, f"ard{i}", [128, 512], F32), Buf()) for i in range(2)])
                rr_r = Ring([(sb(ps_, f"arr{i}", [1, S], BF16), Buf()) for i in range(2)])
                negm = sb(ps_, "negm", [128, 7 * 128], BF16)
                negb = Buf()
                K.dma("pool", negm[:], c_negm[:, :], writes=[negb])
                bq_r = Ring([(sb(ps_, f"abq{i}", [128, NT], F32), Buf()) for i in range(2)])
                tmp_r = Ring([(sb(ps_, f"atm{i}", [128, 128], F32), Buf()) for i in range(2)])
                dmp_r = Ring([(sb(ps_, f"admp{i}", [128, 4, 128], F32), Buf()) for i in range(2)])
                esk = sb(ps_, "esk", [128, 32], F32)
                eskp = sb(ps_, "eskp", [128, 16], F32)
                eskb = Buf()
                vpad = sb(ps_, "vpad", [128, 2, NT, 128], BF16)
                vpb = [Buf(), Buf()]
                hone = sb(ps_, "hone", [128, 2, 128], BF16)
                honb = Buf()
                s_ring = Ring(psf[0:2])
                o_ring = Ring(psf[2:4])
                d_ring = Ring(psf[4:6])

                def finish(pso, pob, psd, pdb, ostage, osb, qb, addcol=None):
                    rd, rdb = rd_r.next()
                    if addcol is not None:
                        K.op("dve", lambda h: h.tensor_scalar(out=rd[:, 0:128], in0=psd[:, 0:128], scalar1=addcol, scalar2=None, op0=ALU.add),
                             reads=[pdb, eskb], writes=[rdb])
                        K.op("dve", lambda h: h.reciprocal(out=rd[:, 0:128], in_=rd[:, 0:128]), reads=[rdb], writes=[rdb])
                    else:
                        K.op("dve", lambda h: h.reciprocal(out=rd[:, 0:128], in_=psd[:, 0:128]), reads=[pdb], writes=[rdb])
                    K.op("dve", lambda h: h.tensor_tensor(out=ostage[:, qb * 128:(qb + 1) * 128], in0=pso[:, 0:128], in1=rd[:, 0:128], op=ALU.mult),
                         reads=[pob, rdb], writes=[osb])

                QW = 512
                for kind in ("mla", "fox"):
                    sc = (192 ** -0.5) if kind == "mla" else (128 ** -0.5)
                    if kind == "mla":
                        K.dma("sp", kr[:], krT[:, :], writes=[krb])
                    for hd in range(16):
                        (qn, qnb), (kn, knb), (v, vb), (ost, osb) = qn_r.next(), kn_r.next(), v_r.next(), o_r.next()
                        if kind == "mla":
                            qr, qrb = qr_r.next()
                            K.dma("sp", qn[:], qnT[hd], writes=[qnb])
                            K.dma("sp", kn[:], knT[hd], writes=[knb])
                            K.dma("sp", qr[:], qrT[hd], writes=[qrb])
                            K.dma("sp", v[:], vm[:, hd * 128:(hd + 1) * 128].rearrange("(n p) d -> p n d", p=128), writes=[vb])
                        else:
                            K.dma("sp", qn[:], fqT[hd], writes=[qnb])
                            K.dma("sp", kn[:], fkT[hd], writes=[knb])
                            K.dma("sp", v[:], fv[:, hd * 128:(hd + 1) * 128].rearrange("(n p) d -> p n d", p=128), writes=[vb])
                            rr, rrb = rr_r.next()
                            K.op("dve", lambda h: h.tensor_scalar(
                                out=rr[0:1, :].rearrange("p (n i) -> p n i", i=128),
                                in0=nmid[0:1, :].rearrange("p (n h) -> p n h", h=16)[:, :, hd:hd + 1].to_broadcast([1, NT, 128]),
                                scalar1=-1.0 / sc, scalar2=None, op0=ALU.mult), reads=[nmb], writes=[rrb])
                        for Q in range(S // QW):
                            (pso, pob), (psd, pdb) = o_ring.next(), d_ring.next()
                            q0 = Q * QW
                            nk = 4 * Q + 4
                            for kt in range(nk):
                                j = kt - 4 * Q
                                k0 = kt * 128
                                pss, psb_ = s_ring.next()
                                mm = [(kn[:, k0:k0 + 128], qn[:, q0:q0 + QW])]
                                rds = [qnb, knb]
                                if kind == "mla":
                                    mm.append((kr[:, k0:k0 + 128], qr[:, q0:q0 + QW]))
                                    rds += [qrb, krb]
                                else:
                                    mm.append((ones_bf[0:1, :], rr[0:1, q0:q0 + QW]))
                                    rds += [rrb, ob]
                                if j >= 0:
                                    mm.append((ident[:], negm[:, (3 - j) * 128:(3 - j) * 128 + QW]))
                                    rds += [cst, negb]
                                fns = [(lambda h, i=i, l_=l_, r_=r_: h.matmul(pss[:, 0:QW], lhsT=l_, rhs=r_, start=(i == 0), stop=(i == len(mm) - 1)))
                                       for i, (l_, r_) in enumerate(mm)]
                                K.op("pe", fns, reads=rds, writes=[psb_])
                                pt, ptb = pt_r.next()
                                if kind == "mla":
                                    K.op("act", lambda h: h.activation(out=pt[:], in_=pss[:, 0:QW], func=AF.Exp, scale=sc), reads=[psb_], writes=[ptb])
                                else:
                                    K.op("act", lambda h: h.activation(out=pt[:], in_=pss[:, 0:QW], func=AF.Exp, scale=sc, bias=ncum[:, kt, hd:hd + 1]),
                                         reads=[psb_, ncb], writes=[ptb])
                                K.op("pe", [lambda h: h.matmul(pso[:, 0:QW], lhsT=v[:, kt, :], rhs=pt[:], start=(kt == 0), stop=(kt == nk - 1)),
                                            lambda h: h.matmul(psd[:, 0:QW], lhsT=ones_bf[:], rhs=pt[:], start=(kt == 0), stop=(kt == nk - 1))],
                                     reads=[vb, ptb, ob], writes=[pob, pdb])
                            rd, rdb = rd_r.next()
                            K.op("dve", lambda h: h.reciprocal(out=rd[:], in_=psd[:, 0:QW]), reads=[pdb], writes=[rdb])
                            K.op("dve", lambda h: h.tensor_tensor(out=ost[:, q0:q0 + QW], in0=pso[:, 0:QW], in1=rd[:], op=ALU.mult),
                                 reads=[pob, rdb], writes=[osb])
                        K.dma("sp", oT[(0 if kind == "mla" else 16) + hd], ost[:], reads=[osb])

                sc = 64 ** -0.5
                K.dma("sp", esk[:], w["sinks"].partition_broadcast(128), writes=[eskb])
                K.op("act", lambda h: h.activation(out=esk[:], in_=esk[:], func=AF.Exp), reads=[eskb], writes=[eskb])
                ev = esk[:].rearrange("p (j two) -> p j two", two=2)
                K.op("dve", lambda h: h.tensor_copy(out=eskp[0:64, :], in_=ev[0:64, :, 0]), reads=[eskb], writes=[eskb])
                K.op("dve", lambda h: h.tensor_copy(out=eskp[64:128, :], in_=ev[64:128, :, 1]), reads=[eskb], writes=[eskb])
                K.op("dve", lambda h: h.memset(hone[:], 0.0), writes=[honb])
                K.op("dve", lambda h: h.memset(hone[:, 0, 0:64], 1.0), writes=[honb])
                K.op("dve", lambda h: h.memset(hone[:, 1, 64:128], 1.0), writes=[honb])
                for g in range(4):
                    kn, knb = kn_r.next()
                    half = g % 2
                    for rr in range(2):
                        K.dma("sp", kn[rr * 64:(rr + 1) * 64, :], skT[g // 2, half * 64:(half + 1) * 64, :], writes=[knb], join=(rr == 1))
                    K.op("dve", lambda h: h.memset(vpad[:], 0.0), writes=vpb)
                    for rr in range(2):
                        K.dma("sp", vpad[:, rr, :, rr * 64:(rr + 1) * 64],
                              sv[:, g * 64:(g + 1) * 64].rearrange("(n p) d -> p n d", p=128), writes=[vpb[rr]])
                    for pj in range(4):
                        pair = g * 4 + pj
                        (qn, qnb), (ost, osb) = qn_r.next(), o_r.next()
                        K.dma("sp", qn[:], sqT[pair], writes=[qnb])
                        dmp, dmpb = dmp_r.next()
                        for hh in range(2):
                            slope = 2.0 ** (-8.0 * (2 * pair + hh + 1) / 32.0)
                            for di, dsel in enumerate((d0, d1)):
                                K.op("dve", lambda h, hh=hh, di=di, dsel=dsel, slope=slope: h.tensor_scalar(
                                    out=dmp[:, hh * 2 + di, :], in0=dsel[:], scalar1=-slope, scalar2=None, op0=ALU.mult),
                                    reads=[cst], writes=[dmpb])
                        for qb in range(NT):
                            (pso, pob), (psd, pdb) = o_ring.next(), d_ring.next()
                            q0 = qb * 128
                            kts = [qb - 1, qb] if qb > 0 else [qb]
                            steps = [(hh, kt) for hh in range(2) for kt in kts]
                            for si, (hh, kt) in enumerate(steps):
                                hidx = 2 * pair + hh
                                slope = 2.0 ** (-8.0 * (hidx + 1) / 32.0)
                                k0 = kt * 128
                                pr = slice(hh * 64, (hh + 1) * 64)
                                pss, psb_ = s_ring.next()
                                K.op("pe", lambda h: h.matmul(pss[:, 0:128], lhsT=kn[pr, k0:k0 + 128], rhs=qn[pr, q0:q0 + 128], start=True, stop=True),
                                     reads=[qnb, knb], writes=[psb_])
                                di = 0 if kt == qb else 1
                                tm, tmb = tmp_r.next()
                                K.op("dve", lambda h: h.scalar_tensor_tensor(out=tm[:], in0=pss[:, 0:128], scalar=sc, in1=dmp[:, hh * 2 + di, :],
                                                                             op0=ALU.mult, op1=ALU.add), reads=[psb_, dmpb], writes=[tmb])
                                pt, ptb = pt_r.next()
                                K.op("act", lambda h: h.activation(out=pt[:, 0:128], in_=tm[:], func=AF.Exp), reads=[tmb], writes=[ptb])
                                K.op("pe", [lambda h: h.matmul(pso[:, 0:128], lhsT=vpad[:, hh, kt, :], rhs=pt[:, 0:128], start=(si == 0), stop=(si == len(steps) - 1)),
                                            lambda h: h.matmul(psd[:, 0:128], lhsT=hone[:, hh, :], rhs=pt[:, 0:128], start=(si == 0), stop=(si == len(steps) - 1))],
                                     reads=[vpb[hh], ptb, honb], writes=[pob, pdb])
                            finish(pso, pob, psd, pdb, ost, osb, qb, addcol=eskp[:, pair:pair + 1])
                        K.dma("sp", oT[32 + pair], ost[:], reads=[osb])
                K.barrier()

            if stop == 3:
                return
            with ExitStack() as ps_:
                OT = sb(ps_, "OT", [128, 48, TB], BF16)
                OTb = Buf()
                MT = sb(ps_, "MT", [128, 32, TB], BF16)
                MTb = Buf()
                wts = Ring([(sb(ps_, f"wt{i}", [128, 32, 512], BF16), Buf()) for i in range(2)])
                gl_r = Ring([(sb(ps_, f"gl{i}", [128, NTB, 512], BF16), Buf()) for i in range(2)])
                macc = sb(ps_, "macc", [128, NTB, 512], F32)
                maccb = [Buf() for _ in range(NTB)]
                mtmp = Ring([(sb(ps_, f"mtmp{i}", [128, 512], F32), Buf()) for i in range(2)])
                mbf = Ring([(sb(ps_, f"mbf{i}", [128, 512], BF16), Buf()) for i in range(2)])
                xr_r = Ring([(sb(ps_, f"xr{i}", [128, 512], F32), Buf()) for i in range(2)])
                for tb in range(NB):
                    t0 = tb * TB
                    for c in range(48):
                        K.dma("sp", OT[:, c, :], oT[c, :, t0:t0 + TB], writes=[OTb], join=(c > 0))
                    for n in range(8):
                        n0 = n * 512
                        chs = []
                        for br in range(3):
                            chs.append(dict(parts=[(w["w_branch"][br * 2048:(br + 1) * 2048, n0:n0 + 512], 0, 512)], ncols=512, br=br))
                        state = {}

                        def epi_b(ch, ti, ps, pb, n=n, n0=n0):
                            br = ch["br"]
                            if ti == 0:
                                state["gl"] = gl_r.next()
                                K.dma("sp", state["gl"][0][:], gt[t0:t0 + TB, br * D + n0:br * D + n0 + 512].rearrange("(i p) c -> p i c", p=128),
                                      writes=[state["gl"][1]])
                            glt, glb = state["gl"]
                            if br == 0:
                                K.op("dve", lambda h: h.tensor_tensor(out=macc[:, ti, :], in0=ps[:], in1=glt[:, ti, :], op=ALU.mult),
                                     reads=[pb, glb], writes=[maccb[ti]])
                            else:
                                t_, tb_ = mtmp.next()
                                K.op("dve", lambda h: h.tensor_tensor(out=t_[:], in0=ps[:], in1=glt[:, ti, :], op=ALU.mult),
                                     reads=[pb, glb], writes=[tb_])
                                K.op("pool", lambda h: h.tensor_tensor(out=macc[:, ti, :], in0=macc[:, ti, :], in1=t_[:], op=ALU.add),
                                     reads=[tb_], writes=[maccb[ti]])
                            if br == 2:
                                o, obf = mbf.next()
                                K.op("act", lambda h: h.activation(out=o[:], in_=macc[:, ti, :], func=AF.Copy), reads=[maccb[ti]], writes=[obf])

                                def dstm(j0, nn, pt, pbuf):
                                    K.op("dve", lambda h: h.tensor_copy(out=MT[:, n * 4:n * 4 + 4, ti * 128:(ti + 1) * 128],
                                                                        in_=pt[:, 0:512].rearrange("p (j t) -> p j t", t=128)),
                                         reads=[pbuf], writes=[MTb])
                                transposes(o, obf, [(j * 128, 128) for j in range(4)], dstm)

                        for ch in chs:
                            br = ch["br"]
                            gemm(OT[:, br * 16:(br + 1) * 16, :], OTb, 16, NTB, [ch], epi_b, wts,
                                 cache=get_cache(f"br{l}", 24, 16), fp=(tb == 0), ci0=n * 3 + br)
                    och = [dict(parts=[(w["w_out"][:, n * 512:(n + 1) * 512], 0, 512)], ncols=512, n0=n * 512) for n in range(8)]

                    def epi_o(ch, ti, ps, pb):
                        r0, n0 = t0 + ti * 128, ch["n0"]
                        xr, xrb = xr_r.next()
                        K.dma("sp", xr[:], x_src[r0:r0 + 128, n0:n0 + 512], writes=[xrb])
                        K.op("dve", lambda h: h.tensor_tensor(out=xr[:], in0=ps[:], in1=xr[:], op=ALU.add), reads=[pb, xrb], writes=[xrb])
                        K.dma("sp", x_dst[r0:r0 + 128, n0:n0 + 512], xr[:], reads=[xrb])

                    gemm(MT, MTb, 32, NTB, och, epi_o, wts, cache=get_cache(f"wo{l}", 8, 32), fp=(tb == 0))
                K.barrier()

        def ffn(l, x_src, x_dst, experts, moe):
            w = W[l]
            with ExitStack() as ps_:
                XT = sb(ps_, "XT", [128, 32, TB], BF16)
                XTb = Buf()
                wts = Ring([(sb(ps_, f"wt{i}", [128, 32, 512], BF16), Buf()) for i in range(2)])
                cmb = sb(ps_, "cmb", [128, NTB, 8], F32)
                cmbb = Buf()
                xacc = [[Buf() for _ in range(8)] for _ in range(NTB)]
                if moe:
                    wrs = sb(ps_, "wrs", [128, 32, 8], F32)
                    wrb = Buf()
                    K.dma("sp", wrs[:], wr.rearrange("(kc p) e -> p kc e", p=128), writes=[wrb])
                    rt = sb(ps_, "rt", [128, 64], F32)
                    rtb = Buf()
                else:
                    K.op("dve", lambda h: h.memset(cmb[:], 1.0), writes=[cmbb])
                for tb in range(NB):
                    t0 = tb * TB

                    def router(ti, xt, xtb, ss, ssb, gbc, gb):
                        K.op("dve", lambda h: h.scalar_tensor_tensor(out=xn32[:], in0=xt[:], scalar=ss[:, 1:2], in1=gbc[:],
                                                                      op0=ALU.mult, op1=ALU.mult), reads=[xtb, ssb, gb], writes=[xn32b])
                        for g0 in range(0, 32, 4):
                            ps, pb = psf_r.next()
                            fns = [(lambda h, j=j: h.transpose(out=ps[:, j * 128:(j + 1) * 128], in_=xn32[:, (g0 + j) * 128:(g0 + j + 1) * 128],
                                                               identity=ident32[:])) for j in range(4)]
                            K.op("pe", fns, reads=[xn32b, cst], writes=[pb])
                            K.op("act", lambda h: h.activation(out=xT32[:, g0:g0 + 4, :], in_=ps[:].rearrange("p (j t) -> p j t", t=128), func=AF.Copy),
                                 reads=[pb], writes=[xT32b])
                        ps, pb = psf_r.next()
                        fns = [(lambda h, kc=kc: h.matmul(ps[:, 0:8], lhsT=xT32[:, kc, :], rhs=wrs[:, kc, :], start=(kc == 0), stop=(kc == 31)))
                               for kc in range(32)]
                        K.op("pe", fns, reads=[xT32b, wrb], writes=[pb])
                        lg, m1, e1, l2, m2, e2, wa, wb_ = (rt[:, 0:8], rt[:, 8:9], rt[:, 16:24], rt[:, 24:32], rt[:, 9:10], rt[:, 32:40],
                                                           rt[:, 10:11], rt[:, 11:12])
                        K.op("dve", lambda h: h.tensor_copy(out=lg, in_=ps[:, 0:8]), reads=[pb], writes=[rtb])
                        K.op("dve", lambda h: h.tensor_reduce(out=m1, in_=lg, axis=mybir.AxisListType.X, op=ALU.max), reads=[rtb], writes=[rtb])
                        K.op("dve", lambda h: h.tensor_scalar(out=e1, in0=lg, scalar1=m1, scalar2=None, op0=ALU.is_equal), reads=[rtb], writes=[rtb])
                        K.op("dve", lambda h: h.scalar_tensor_tensor(out=l2, in0=e1, scalar=-1e30, in1=lg, op0=ALU.mult, op1=ALU.add),
                             reads=[rtb], writes=[rtb])
                        K.op("dve", lambda h: h.tensor_reduce(out=m2, in_=l2, axis=mybir.AxisListType.X, op=ALU.max), reads=[rtb], writes=[rtb])
                        K.op("dve", lambda h: h.tensor_scalar(out=e2, in0=l2, scalar1=m2, scalar2=None, op0=ALU.is_equal), reads=[rtb], writes=[rtb])
                        K.op("dve", lambda h: h.tensor_tensor(out=wb_, in0=m2, in1=m1, op=ALU.subtract), reads=[rtb], writes=[rtb])
                        K.op("act", lambda h: h.activation(out=wb_, in_=wb_, func=AF.Sigmoid), reads=[rtb], writes=[rtb])
                        K.op("dve", lambda h: h.tensor_scalar(out=wa, in0=wb_, scalar1=-1.0, scalar2=1.0, op0=ALU.mult, op1=ALU.add),
                             reads=[rtb], writes=[rtb])
                        K.op("dve", lambda h: h.tensor_scalar(out=e1, in0=e1, scalar1=wa, scalar2=None, op0=ALU.mult), reads=[rtb], writes=[rtb])
                        K.op("dve", lambda h: h.scalar_tensor_tensor(out=cmb[:, ti, :], in0=e2, scalar=wb_, in1=e1, op0=ALU.mult, op1=ALU.add),
                             reads=[rtb], writes=[cmbb])

                    with ExitStack() as ns:
                        if moe:
                            xn32 = sb(ns, "xn32", [128, D], F32)
                            xn32b = Buf()
                            xT32 = sb(ns, "xT32", [128, 32, 128], F32)
                            xT32b = Buf()
                        norm_rows(ns, x_src, w["g_ffn"], tb, XT, XTb, "f", want32=router if moe else None)
                        K.barrier()
                    es2 = ExitStack()
                    HT = sb(es2, "HT", [128, 32, TB], BF16)
                    HTb = Buf()
                    sil = Ring([(sb(es2, f"sil{i}", [128, TB], F32), Buf()) for i in range(2)])
                    xr_r = Ring([(sb(es2, f"xr{i}", [128, 512], F32), Buf()) for i in range(3)])
                    for ei, (wg_ap, wu_ap, wd_ap, ff) in enumerate(experts):
                        KCH = ff // 128
                        gcache = get_cache(f"gu{l}_{ei}", ff // 256, 32)
                        for c in range(ff // 256):
                            wt, wb = wts.next()
                            if tb > 0:
                                K.dma("sp", wt[:, 0:32, :], gcache[0][c], reads=[gcache[1][c]], writes=[wb])
                            else:
                                first = True
                                for (ap, off) in ((wg_ap[:, c * 256:(c + 1) * 256], 0), (wu_ap[:, c * 256:(c + 1) * 256], 256)):
                                    src = ap.rearrange("(kc p) c -> p kc c", p=128)
                                    K.dma("pool", wt[:, 0:32, off:off + 256], src[:, 0:32, :], writes=[wb], join=not first)
                                    first = False
                                K.dma("sp", gcache[0][c], wt[:, 0:32, :], reads=[wb], writes=[gcache[1][c]])
                            for j in range(2):
                                (psg, pgb), (psu, pub) = psf_r.next(), psf_r.next()
                                fns = [(lambda h, kc=kc: h.matmul(psg[:, 0:TB], lhsT=wt[:, kc, j * 128:(j + 1) * 128], rhs=XT[:, kc, 0:TB],
                                                                  start=(kc == 0), stop=(kc == 31))) for kc in range(32)]
                                K.op("pe", fns, reads=[XTb, wb], writes=[pgb])
                                fns = [(lambda h, kc=kc: h.matmul(psu[:, 0:TB], lhsT=wt[:, kc, 256 + j * 128:256 + (j + 1) * 128], rhs=XT[:, kc, 0:TB],
                                                                  start=(kc == 0), stop=(kc == 31))) for kc in range(32)]
                                K.op("pe", fns, reads=[XTb, wb], writes=[pub])
                                s_, sb_ = sil.next()
                                K.op("act", lambda h: h.activation(out=s_[:], in_=psg[:, 0:TB], func=AF.Silu), reads=[pgb], writes=[sb_])
                                K.op("dve", lambda h: h.tensor_tensor(out=HT[:, 2 * c + j, :], in0=psu[:, 0:TB], in1=s_[:], op=ALU.mult),
                                     reads=[pub, sb_], writes=[HTb])
                        dch = [dict(parts=[(wd_ap[:, n * 512:(n + 1) * 512], 0, 512)], ncols=512, n=n) for n in range(8)]

                        def epi_d(ch, ti, ps, pb, ei=ei):
                            n = ch["n"]
                            r0, n0 = t0 + ti * 128, n * 512
                            xr, xrb = xr_r.next()
                            src = x_src if ei == 0 else x_dst
                            K.dma("sp", xr[:], src[r0:r0 + 128, n0:n0 + 512], reads=[xacc[ti][n]], writes=[xrb])
                            col = cmb[:, ti, ei:ei + 1] if moe else cmb[:, ti, 0:1]
                            K.op("dve", lambda h: h.scalar_tensor_tensor(out=xr[:], in0=ps[:], scalar=col, in1=xr[:], op0=ALU.mult, op1=ALU.add),
                                 reads=[pb, xrb, cmbb], writes=[xrb])
                            K.dma("sp", x_dst[r0:r0 + 128, n0:n0 + 512], xr[:], reads=[xrb], writes=[xacc[ti][n]])

                        gemm(HT[:, 0:KCH, :], HTb, KCH, NTB, dch, epi_d, wts, cache=get_cache(f"dn{l}_{ei}", 8, KCH), fp=(tb == 0))
                    K.barrier()
                    es2.close()
                K.barrier()

        def final_norm(x_src):
            with ExitStack() as ps_:
                gbc = sb(ps_, "gbcF", [128, D], F32)
                gb = Buf()
                K.dma("sp", gbc[:], g_final.partition_broadcast(128), writes=[gb])
                xt_r = Ring([(sb(ps_, f"xtF{i}", [128, D], F32), Buf()) for i in range(2)])
                junk = sb(ps_, "junkF", [128, D], BF16)
                jb = Buf()
                ss_r = Ring([(sb(ps_, f"ssF{i}", [128, 2], F32), Buf()) for i in range(2)])
                for ti in range(NT):
                    xt, xtb = xt_r.next()
                    ss, ssb = ss_r.next()
                    K.dma("sp", xt[:], x_src[ti * 128:(ti + 1) * 128, :], writes=[xtb])
                    K.op("dve", lambda h: h.memset(ss[:], 0.0), writes=[ssb])
                    K.op("act", lambda h: h.activation(out=junk[:], in_=xt[:], func=AF.Square, accum_out=ss[:, 0:1]),
                         reads=[xtb], writes=[jb, ssb])
                    K.op("act", lambda h: h.activation(out=ss[:, 1:2], in_=ss[:, 0:1], func=AF.Ln, scale=1.0 / D, bias=EPS),
                         reads=[ssb], writes=[ssb])
                    K.op("act", lambda h: h.activation(out=ss[:, 1:2], in_=ss[:, 1:2], func=AF.Exp, scale=-0.5),
                         reads=[ssb], writes=[ssb])
                    K.op("dve", lambda h: h.scalar_tensor_tensor(out=xt[:], in0=xt[:], scalar=ss[:, 1:2], in1=gbc[:], op0=ALU.mult, op1=ALU.mult),
                         reads=[xtb, ssb, gb], writes=[xtb])
                    K.dma("sp", y_out[ti * 128:(ti + 1) * 128, :], xt[:], reads=[xtb])
                K.barrier()

        cur = x_in
        scr = [xa, xb, xc]
        si = 0
        for l in range(n_layers):
            nxt = scr[si % 3]; si += 1
            if stop != 11:
                mixer(l, cur, nxt)
            if stop == 0:
                cur = nxt
            if do_ffn[l]:
                nxt = scr[si % 3]; si += 1
                if l == 0:
                    ex = [(wdg[:, o:o + f], wdu[:, o:o + f], wdd[o:o + f, :], f) for (o, f) in ((0, 4096), (4096, 4096), (8192, 4096), (12288, 2048))]
                    ffn(l, cur, nxt, ex, moe=False)
                else:
                    ex = [(weg[e], weu[e], wed[e], 4096) for e in range(8)]
                    ffn(l, cur, nxt, ex, moe=True)
                cur = nxt
        if do_final:
            final_norm(cur)
        else:
            with ExitStack() as ps_:
                xt = sb(ps_, "xtD", [128, D], F32)
                xtb = Buf()
                for ti in range(NT):
                    K.dma("sp", xt[:], cur[ti * 128:(ti + 1) * 128, :], writes=[xtb])
                    K.dma("sp", y_out[ti * 128:(ti + 1) * 128, :], xt[:], reads=[xtb])
                K.barrier()
    return nc


def consts(S):
    k = np.arange(128)[:, None]
    q = np.arange(128)[None, :]
    tri = (q >= k).astype(np.float32)
    d0 = np.where(q >= k, (q - k).astype(np.float32), np.float32(1e9)).astype(np.float32)
    d1 = np.where(q < k, (q + 128 - k).astype(np.float32), np.float32(1e9)).astype(np.float32)
    sel = np.zeros((128, 128), np.float32)
    sel[64, :] = 1.0
    half = 32
    inv = (np.float32(10000.0) ** (-np.arange(half, dtype=np.float32) / np.float32(half))).astype(np.float32)
    ang = np.arange(S, dtype=np.float32)[:, None] * inv[None, :]
    rope = np.concatenate([np.cos(ang), np.sin(ang)], axis=1).astype(np.float32)
    NEG = np.float32(-60000.0)
    negtri = np.where(q >= k, np.float32(0.0), NEG).astype(np.float32)
    negm = np.concatenate([np.full((128, 384), NEG, np.float32), negtri, np.zeros((128, 384), np.float32)], axis=1)
    return dict(c_ident=np.eye(128, dtype=np.float32), c_tri=tri, c_d0=d0, c_d1=d1, c_sel=sel, c_rope=rope, c_negm=negm)


def make_in_map(inp, b, S, n_layers=2, do_ffn=(True, True)):
    m = {"x": np.ascontiguousarray(inp["x"][b, :S])}
    names = dict(g_mix="g_mix_norm", w_in="w_in", b_forget="b_forget", b_gate="b_gate", g_q="g_q_norm", g_kv="g_kv_norm",
                 w_uq="w_uq", w_ukv="w_ukv", sinks="sinks", w_branch="w_branch", w_out="w_out", g_ffn="g_ffn_norm")
    for l in range(n_layers):
        for k, v in names.items():
            m[f"{k}{l}"] = inp[v][l]
    if n_layers > 0 and do_ffn[0]:
        m["w_dg"], m["w_du"], m["w_dd"] = inp["w_dense_gate"][0], inp["w_dense_up"][0], inp["w_dense_down"][0]
    if n_layers > 1 and do_ffn[1]:
        m["w_router"] = inp["w_router"][0]
        m["w_eg"], m["w_eu"], m["w_ed"] = inp["w_exp_gate"][0], inp["w_exp_up"][0], inp["w_exp_down"][0]
    m["g_final"] = inp["g_final"]
    m.update(consts(S))
    return m


def kernel(**inputs):
    inp = {k: np.asarray(v) for k, v in inputs.items()}
    B, S, _ = inp["x"].shape
    nc = build(S)
    in_maps = [make_in_map(inp, b, S) for b in range(B)]
    res = run_bass_kernel_spmd(nc, in_maps, core_ids=list(range(B)))
    return np.stack([np.asarray(r["y"]) for r in res.results], axis=0).astype(np.float32)
```
